# Optimizing a Trainium2 kernel written in Bass

```python
import math
import jax, jax.numpy as jnp
from jax import lax
import numpy as np

D_MODEL = 1024
BATCH = 2
SEQ = 16384
DEPTH = 1

A_HEADS = 8
A_DK = 128
A_DV = D_MODEL // A_HEADS
A_QK = A_HEADS * A_DK
A_V = A_HEADS * A_DV
B_HEADS = 4
B_DK = (D_MODEL // 2) // B_HEADS
B_DV = D_MODEL // B_HEADS
B_QK = B_HEADS * B_DK
B_V = B_HEADS * B_DV
GLA_RANK = 16
GLA_TAU = 16.0
CHUNK = 64
N_EXPERTS = 32
TOP_K = 4
D_FF = D_MODEL
SWIGLU_LIMIT = 7.0
SWIGLU_ALPHA = 1.702
MOE_BLOCK = 128
PLE_DIM = 256
LN_EPS = 1e-5
RMS_EPS = 1e-6
DN_ALPHA = (2.0 * DEPTH) ** 0.25
DN_BETA = (8.0 * DEPTH) ** -0.25
IN_SIZES = (A_QK, A_QK, A_V, A_V, B_QK, B_QK, B_V, B_V, GLA_RANK, D_MODEL, D_MODEL)
D_IN = sum(IN_SIZES)

kernel_name = 'hybrid_hgrn2_gla_moe_block'


def layer_norm(x, g, b):
    xf = x.astype(jnp.float32)
    mu = jnp.mean(xf, axis=-1, keepdims=True)
    var = jnp.mean(jnp.square(xf - mu), axis=-1, keepdims=True)
    y = (xf - mu) * lax.rsqrt(var + LN_EPS) * g.astype(jnp.float32) + b.astype(jnp.float32)
    return y.astype(x.dtype)


def head_rms_norm(o, g):
    return o * lax.rsqrt(jnp.mean(jnp.square(o), axis=-1, keepdims=True) + RMS_EPS) * g.astype(jnp.float32)


def chunk_gated_linear_attention(q, k, v, log_g):
    bsz, s, h, dk = q.shape
    dv = v.shape[-1]
    n = s // CHUNK

    def to_chunks(t):
        return t.astype(jnp.float32).reshape(bsz, n, CHUNK, h, t.shape[-1]).transpose(1, 0, 3, 2, 4)

    qc, kc, vc, gc = to_chunks(q), to_chunks(k), to_chunks(v), to_chunks(log_g)
    causal = jnp.tril(jnp.ones((CHUNK, CHUNK), dtype=bool))[:, :, None]

    def step(state, inp):
        qi, ki, vi, gi = inp
        b = jnp.cumsum(gi, axis=2)
        o_inter = jnp.einsum('bhcd,bhde->bhce', qi * jnp.exp(b), state)
        rel = b[:, :, :, None, :] - b[:, :, None, :, :]
        decay = jnp.exp(jnp.where(causal, rel, -jnp.inf))
        scores = jnp.einsum('bhid,bhjd,bhijd->bhij', qi, ki, decay)
        o_intra = jnp.einsum('bhij,bhje->bhie', scores, vi)
        b_last = b[:, :, -1:, :]
        k_dec = ki * jnp.exp(b_last - b)
        state = jnp.exp(b_last[:, :, 0, :])[..., None] * state + jnp.einsum('bhjd,bhje->bhde', k_dec, vi)
        return state, o_inter + o_intra

    state0 = jnp.zeros((bsz, h, dk, dv), jnp.float32)
    _, out = lax.scan(step, state0, (qc, kc, vc, gc))
    return out.transpose(1, 0, 3, 2, 4).reshape(bsz, s, h, dv)


def hybrid_mixer(x, lb, w_in, w_gla_up, b_gla_up, norm_a_g, norm_b_g, w_proj_a, w_proj_b, w_out):
    bsz, s, _ = x.shape
    f32 = jnp.float32
    offsets = [int(o) for o in np.cumsum(IN_SIZES)[:-1]]
    z = x @ w_in
    q_a, f_a, i_a, g_a, q_b, k_b, v_b, r_b, lr_b, gate_a, gate_b = jnp.split(z, offsets, axis=-1)

    fz = f_a.astype(f32)
    forget = lb + (1.0 - lb) * jax.nn.sigmoid(fz)
    k_a = (1.0 - lb) * jax.nn.sigmoid(-fz)
    qa = jax.nn.silu(q_a.astype(f32)) * (A_DK ** -0.5)
    o_a = chunk_gated_linear_attention(
        qa.reshape(bsz, s, A_HEADS, A_DK), k_a.reshape(bsz, s, A_HEADS, A_DK),
        i_a.reshape(bsz, s, A_HEADS, A_DV), jnp.log(forget).reshape(bsz, s, A_HEADS, A_DK))
    o_a = head_rms_norm(o_a, norm_a_g).reshape(bsz, s, A_V) * jax.nn.silu(g_a.astype(f32))

    log_g = jax.nn.log_sigmoid((lr_b @ w_gla_up + b_gla_up).astype(f32)) / GLA_TAU
    qb = q_b.astype(f32) * (B_DK ** -0.5)
    o_b = chunk_gated_linear_attention(
        qb.reshape(bsz, s, B_HEADS, B_DK), k_b.reshape(bsz, s, B_HEADS, B_DK),
        v_b.reshape(bsz, s, B_HEADS, B_DV), log_g.reshape(bsz, s, B_HEADS, B_DK))
    o_b = head_rms_norm(o_b, norm_b_g).reshape(bsz, s, B_V) * jax.nn.silu(r_b.astype(f32))

    ya = o_a.astype(x.dtype) @ w_proj_a
    yb = o_b.astype(x.dtype) @ w_proj_b
    merged = jax.nn.sigmoid(gate_a) * ya + jax.nn.sigmoid(gate_b) * yb
    return merged @ w_out


def moe_ffn(x, w_router, b_router, w_gate_up, b_gate_up, w_down, b_down):
    bsz, s, d = x.shape
    t = bsz * s
    xt = x.reshape(t, d)
    logits = (xt @ w_router).astype(jnp.float32) + b_router.astype(jnp.float32)
    top_logit, top_e = lax.top_k(logits, TOP_K)
    top_w = jax.nn.softmax(top_logit, axis=-1)
    n_assign = t * TOP_K
    flat_e = top_e.reshape(-1)
    flat_tok = jnp.arange(n_assign, dtype=jnp.int32) // TOP_K
    flat_w = top_w.reshape(-1)
    order = jnp.argsort(flat_e)
    sorted_e = flat_e[order]
    counts = jnp.bincount(flat_e, length=N_EXPERTS)
    padded = (counts + MOE_BLOCK - 1) // MOE_BLOCK * MOE_BLOCK
    start = jnp.cumsum(counts) - counts
    pend = jnp.cumsum(padded)
    pstart = pend - padded
    rank = jnp.arange(n_assign, dtype=jnp.int32) - start[sorted_e]
    dest = pstart[sorted_e] + rank
    n_blocks = (n_assign + MOE_BLOCK - 1) // MOE_BLOCK + N_EXPERTS
    n_rows = n_blocks * MOE_BLOCK
    row_tok = jnp.full((n_rows,), t, jnp.int32).at[dest].set(flat_tok[order])
    row_w = jnp.zeros((n_rows,), jnp.float32).at[dest].set(flat_w[order])
    block_e = jnp.minimum(
        jnp.searchsorted(pend, jnp.arange(n_blocks, dtype=jnp.int32) * MOE_BLOCK, side='right'),
        N_EXPERTS - 1)
    x_pad = jnp.concatenate([xt, jnp.zeros((1, d), xt.dtype)], axis=0)

    def expert_block(args):
        toks, e = args
        xb = x_pad[toks]
        hgu = xb @ w_gate_up[e] + b_gate_up[e]
        gate = jnp.minimum(hgu[:, :D_FF], SWIGLU_LIMIT)
        up = jnp.clip(hgu[:, D_FF:], -SWIGLU_LIMIT, SWIGLU_LIMIT)
        act = (up + 1.0) * gate * jax.nn.sigmoid(SWIGLU_ALPHA * gate)
        return act @ w_down[e] + b_down[e]

    out = lax.map(expert_block, (row_tok.reshape(n_blocks, MOE_BLOCK), block_e))
    out = out.reshape(n_rows, d) * row_w[:, None].astype(out.dtype)
    y = jax.ops.segment_sum(out, row_tok, num_segments=t + 1)[:t]
    return y.reshape(bsz, s, d)


def setup_inputs(seed: int = 0) -> dict:
    key = jax.random.key(seed)
    ks = jax.random.split(key, 32)
    f32 = jnp.float32
    L = DEPTH
    D = D_MODEL

    def nrm(k, shape, scale):
        return jax.random.normal(k, shape, f32) * scale

    col_scale = jnp.concatenate([
        jnp.full((sz,), DN_BETA if idx in (2, 6) else 1.0, f32) for idx, sz in enumerate(IN_SIZES)])
    return {
        'x': nrm(ks[0], (BATCH, SEQ, D), 1.0),
        'p': nrm(ks[1], (DEPTH, BATCH, SEQ, PLE_DIM), 1.0),
        'emb_ln_g': 1.0 + nrm(ks[2], (D,), 0.01),
        'emb_ln_b': nrm(ks[3], (D,), 0.01),
        'hgrn_lb': nrm(ks[4], (DEPTH + 1, A_QK), 0.1),
        'w_in': nrm(ks[5], (L, D, D_IN), D ** -0.5) * col_scale,
        'w_gla_up': nrm(ks[6], (L, GLA_RANK, B_QK), GLA_RANK ** -0.5),
        'b_gla_up': nrm(ks[7], (L, B_QK), 0.1),
        'norm_a_g': 1.0 + nrm(ks[8], (L, A_DV), 0.01),
        'norm_b_g': 1.0 + nrm(ks[9], (L, B_DV), 0.01),
        'w_proj_a': nrm(ks[10], (L, A_V, D), A_V ** -0.5 * DN_BETA),
        'w_proj_b': nrm(ks[11], (L, B_V, D), B_V ** -0.5 * DN_BETA),
        'w_out': nrm(ks[12], (L, D, D), D ** -0.5 * DN_BETA),
        'ln_mix_g': 1.0 + nrm(ks[13], (L, D), 0.01),
        'ln_mix_b': nrm(ks[14], (L, D), 0.01),
        'w_router': nrm(ks[15], (L, D, N_EXPERTS), D ** -0.5),
        'b_router': nrm(ks[16], (L, N_EXPERTS), 0.01),
        'w_gate_up': nrm(ks[17], (L, N_EXPERTS, D, 2 * D_FF), D ** -0.5),
        'b_gate_up': nrm(ks[18], (L, N_EXPERTS, 2 * D_FF), 0.01),
        'w_down': nrm(ks[19], (L, N_EXPERTS, D_FF, D), D_FF ** -0.5 * DN_BETA),
        'b_down': nrm(ks[20], (L, N_EXPERTS, D), 0.01),
        'w_ple_gate': nrm(ks[21], (L, D, D), D ** -0.5),
        'w_ple_proj': nrm(ks[22], (L, PLE_DIM, D), PLE_DIM ** -0.5 * DN_BETA),
        'ln_moe_g': 1.0 + nrm(ks[23], (L, D), 0.01),
        'ln_moe_b': nrm(ks[24], (L, D), 0.01),
    }


def reference(x, p, emb_ln_g, emb_ln_b, hgrn_lb, w_in, w_gla_up, b_gla_up, norm_a_g, norm_b_g,
              w_proj_a, w_proj_b, w_out, ln_mix_g, ln_mix_b, w_router, b_router, w_gate_up,
              b_gate_up, w_down, b_down, w_ple_gate, w_ple_proj, ln_moe_g, ln_moe_b):
    h = layer_norm(x, emb_ln_g, emb_ln_b)
    lb_table = jnp.cumsum(jax.nn.softmax(hgrn_lb.astype(jnp.float32), axis=0), axis=0)
    for i in range(DEPTH):
        y = hybrid_mixer(h, lb_table[i], w_in[i], w_gla_up[i], b_gla_up[i], norm_a_g[i], norm_b_g[i],
                         w_proj_a[i], w_proj_b[i], w_out[i])
        h = layer_norm(DN_ALPHA * h + y, ln_mix_g[i], ln_mix_b[i])
        y = moe_ffn(h, w_router[i], b_router[i], w_gate_up[i], b_gate_up[i], w_down[i], b_down[i])
        ple = jax.nn.sigmoid(h @ w_ple_gate[i]) * (p[i].astype(h.dtype) @ w_ple_proj[i])
        h = layer_norm(DN_ALPHA * h + y + ple, ln_moe_g[i], ln_moe_b[i])
    return h
```

```python
import os
import numpy as np
import concourse.bass as bass
import concourse.mybir as mybir
from concourse.bass_utils import run_bass_kernel_spmd

F32 = mybir.dt.float32
BF16 = mybir.dt.bfloat16
I32 = mybir.dt.int32
AF = mybir.ActivationFunctionType
ALU = mybir.AluOpType
AX = mybir.AxisListType

P = 128
D = 1024
SEQ = 16384
NCORES = 8
TSEG = SEQ // 4
NEXP = 32
CAP = 640
NSLOT = NEXP * CAP
LN_EPS = 1e-5
RMS_EPS = 1e-6
DN_ALPHA = 2.0 ** 0.25
SW_LIMIT = 7.0
SW_ALPHA = 1.702


class Buf:
    __slots__ = ("name", "w", "r", "excl")

    def __init__(self, name=""):
        self.name = name
        self.w = {}
        self.r = {}
        self.excl = name.startswith("ps")


class Sched:
    def __init__(self, nc, n_dma_sems=24, self_sync=True):
        self.nc = nc
        self.self_sync = self_sync
        self.eng = {"pe": nc.tensor, "act": nc.scalar, "dve": nc.vector, "pool": nc.gpsimd, "sp": nc.sync}
        self.sems = {}
        self.cnt = {}
        self.waited = {e: {} for e in self.eng}
        self._ctx = []
        self.esem = {}
        for e in self.eng:
            sid = "E_" + e
            self._mk(sid)
            self.esem[e] = sid
        self.dsems = {"sp": [], "pool": []}
        self.dptr = {"sp": 0, "pool": 0}
        for q in ("sp", "pool"):
            for i in range(n_dma_sems):
                sid = "D_%s_%d" % (q, i)
                self._mk(sid)
                self.dsems[q].append(sid)
        self.ninst = {e: 0 for e in self.eng}
        self.nops = 0
        self.limit = int(os.environ.get("KSTOP", "0")) or None
        self._rec = None

    def begin_record(self):
        self._rec = []

    def end_record(self):
        r, self._rec = self._rec, None
        return r

    def play(self, *lists):
        lists = [l for l in lists if l]
        idx = [0] * len(lists)
        while True:
            best, bf = -1, 2.0
            for i, l in enumerate(lists):
                if idx[i] < len(l):
                    f = (idx[i] + 0.5) / len(l)
                    if f < bf:
                        best, bf = i, f
            if best < 0:
                break
            kind, args = lists[best][idx[best]]
            idx[best] += 1
            getattr(self, kind)(*args)

    def _skip(self):
        self.nops += 1
        return self.limit is not None and self.nops > self.limit

    def _mk(self, sid):
        cm = self.nc.semaphore(sid)
        h = cm.__enter__()
        self._ctx.append(cm)
        self.sems[sid] = h
        self.cnt[sid] = 0

    def close(self):
        for cm in reversed(self._ctx):
            cm.__exit__(None, None, None)

    def _need(self, e, deps):
        own = self.esem[e]
        for sid, val in deps.items():
            if sid == own and (e == "pe" or not self.self_sync):
                continue
            if self.waited[e].get(sid, 0) >= val:
                continue
            self.eng[e].wait_ge(self.sems[sid], val)
            self.waited[e][sid] = val
            self.ninst[e] += 1

    @staticmethod
    def _deps(reads, writes):
        d = {}
        for b in reads:
            for sid, v in b.w.items():
                if d.get(sid, 0) < v:
                    d[sid] = v
        for b in writes:
            for sid, v in b.w.items():
                if d.get(sid, 0) < v:
                    d[sid] = v
            for sid, v in b.r.items():
                if d.get(sid, 0) < v:
                    d[sid] = v
        return d

    @staticmethod
    def _mark(sid, val, reads, writes):
        for b in reads:
            if b.r.get(sid, 0) < val:
                b.r[sid] = val
        for b in writes:
            if b.w.get(sid, 0) < val:
                b.w[sid] = val

    def op(self, e, fn, reads=(), writes=()):
        if self._rec is not None:
            self._rec.append(("op", (e, fn, list(reads), list(writes))))
            return None
        if self._skip():
            return None
        if os.environ.get("KLIST"):
            print("OP", self.nops, e, [b.name for b in reads], "->", [b.name for b in writes])
        ex = [b for b in reads if b.excl]
        if ex:
            writes = list(writes) + ex
        self._need(e, self._deps(reads, writes))
        inst = fn(self.eng[e])
        sid = self.esem[e]
        self.cnt[sid] += 1
        inst.then_inc(self.sems[sid], 1)
        self._mark(sid, self.cnt[sid], reads, writes)
        self.ninst[e] += 1
        return inst

    def group(self, e, fns, reads=(), writes=()):
        self._need(e, self._deps(reads, writes))
        inst = None
        for fn in fns:
            inst = fn(self.eng[e])
            self.ninst[e] += 1
        sid = self.esem[e]
        self.cnt[sid] += 1
        inst.then_inc(self.sems[sid], 1)
        self._mark(sid, self.cnt[sid], reads, writes)
        return inst

    def dma(self, q, fn, reads=(), writes=()):
        if self._rec is not None:
            self._rec.append(("dma", (q, fn, list(reads), list(writes))))
            return None
        if self._skip():
            return None
        if os.environ.get("KLIST"):
            print("DMA", self.nops, q, [b.name for b in reads], "->", [b.name for b in writes])
        sems = self.dsems[q]
        sid = sems[self.dptr[q] % len(sems)]
        self.dptr[q] += 1
        deps = self._deps(reads, writes)
        if self.cnt[sid] > 0:
            deps[sid] = max(deps.get(sid, 0), self.cnt[sid])
        self._need(q, deps)
        inst = fn(self.eng[q])
        self.cnt[sid] += 16
        inst.then_inc(self.sems[sid], 16)
        self._mark(sid, self.cnt[sid], reads, writes)
        self.ninst[q] += 1
        return inst

    def cc(self, fn, reads=(), writes=()):
        sid = "CC"
        if sid not in self.sems:
            self._mk(sid)
        self._need("pool", self._deps(reads, writes))
        inst = fn(self.eng["pool"])
        self.cnt[sid] += 1
        inst.then_inc(self.sems[sid], 1)
        self._mark(sid, self.cnt[sid], reads, writes)
        return inst

    def barrier(self, engines=None):
        engines = engines or list(self.eng)
        deps = {sid: v for sid, v in self.cnt.items() if v > 0}
        for e in engines:
            own = self.esem[e]
            for sid, val in deps.items():
                if sid == own:
                    continue
                if self.waited[e].get(sid, 0) >= val:
                    continue
                self.eng[e].wait_ge(self.sems[sid], val)
                self.waited[e][sid] = val


class Ctx:
    def __init__(self, nc, S):
        self.nc = nc
        self.S = S
        self.stack = []

    def sb(self, name, shape, dt):
        cm = self.nc.sbuf_tensor(name, shape, dt)
        t = cm.__enter__()
        self.stack.append(cm)
        return t

    def ps(self, name, shape, dt):
        cm = self.nc.psum_tensor(name, shape, dt)
        t = cm.__enter__()
        self.stack.append(cm)
        return t

    def mark(self):
        return len(self.stack)

    def release(self, mark):
        while len(self.stack) > mark:
            self.stack.pop().__exit__(None, None, None)


def recip1p(S, eng, out, e_ap, tmp, rb, wb, tb, negones=None):
    if eng == "act":
        S.op("act", lambda a: a.activation(out=tmp, in_=e_ap, func=AF.Ln, bias=1.0), reads=rb, writes=[tb])
        S.op("act", lambda a: a.activation(out=out, in_=tmp, func=AF.Exp, scale=-1.0), reads=[tb], writes=wb)
    elif eng == "dve":
        S.op("dve", lambda v: v.tensor_scalar(out=tmp, in0=e_ap, scalar1=1.0, scalar2=None, op0=ALU.add), reads=rb, writes=[tb])
        S.op("dve", lambda v: v.reciprocal(out=out, in_=tmp), reads=[tb], writes=wb)
    else:
        S.op("pool", lambda g: g.tensor_scalar(out=tmp, in0=e_ap, scalar1=1.0, scalar2=None, op0=ALU.add), reads=rb, writes=[tb])
        S.op("pool", lambda g: g.tensor_tensor(out=out, in0=tmp, in1=negones, op=ALU.pow), reads=[tb], writes=wb)


def layernorm_stats(S, x_ap, st, mv, sc, B_x, B_st, eps):
    S.op("dve", lambda v: v.bn_stats(out=st[:, 0:6], in_=x_ap[:, 0:512]), reads=[B_x], writes=[B_st])
    S.op("dve", lambda v: v.bn_stats(out=st[:, 6:12], in_=x_ap[:, 512:1024]), reads=[B_x], writes=[B_st])
    S.op("dve", lambda v: v.bn_aggr(out=mv[:, 0:2], in_=st[:, 0:12]), reads=[B_st], writes=[B_st])
    S.op("act", lambda a: a.activation(out=mv[:, 2:3], in_=mv[:, 1:2], func=AF.Ln, bias=eps), reads=[B_st], writes=[B_st])
    S.op("act", lambda a: a.activation(out=sc[:, 0:1], in_=mv[:, 2:3], func=AF.Exp, scale=-0.5), reads=[B_st], writes=[B_st])
    S.op("dve", lambda v: v.tensor_scalar(out=sc[:, 1:2], in0=mv[:, 0:1], scalar1=sc[:, 0:1], scalar2=-1.0,
                                          op0=ALU.mult, op1=ALU.mult), reads=[B_st], writes=[B_st])


def build_program(n_tiles_p1=SEQ // P, n_tiles_p2=TSEG // P, do_p1=True, do_p2=True, debug=False):
    nc = bass.Bass("TRN2", target_bir_lowering=False)
    S = Sched(nc)
    C = Ctx(nc, S)

    def din(name, shape, dt=F32):
        return nc.dram_tensor(name, shape, dt, kind="ExternalInput").ap()

    xb = din("xb", [SEQ, D])
    w1 = din("w1", [D, 1808])
    vec = din("vec", [16, D])
    wgu_in = din("wgu", [16, 128])
    if do_p2:
        xseg = din("xseg", [TSEG, D])
        pseg = din("pseg", [TSEG, 256])
        wgate = din("wgate", [D, 2048])
        wpa = din("wpa", [D, D])
        wpb = din("wpb", [D, D])
        wout = din("wout", [D, D])
        wr = din("wr", [D, NEXP])
        wplg = din("wplg", [D, D])
        wplp = din("wplp", [256, D])
        wgup = din("wgup", [NEXP, D, 2 * D])
        bgup = din("bgup", [NEXP, 2 * D])
        wdn = din("wdn", [NEXP, D, D])
        bdn = din("bdn", [NEXP, D])
    if do_p2:
        out = nc.dram_tensor("out", [TSEG, D], F32, kind="ExternalOutput").ap()
    oex_t = nc.dram_tensor("oex", [SEQ, 512], BF16)
    oex = oex_t.ap()
    if do_p2:
        oga_t = nc.dram_tensor("oga", [NCORES * SEQ, 512], BF16)
        oga = oga_t.ap()
    B_oex = Buf("oex")
    B_oga = Buf("oga")
    if debug:
        dbg_o = nc.dram_tensor("dbg_o", [SEQ, 512], BF16, kind="ExternalOutput").ap()

    ident = C.sb("ident", [P, P], F32)
    identb = C.sb("identb", [P, P], BF16)
    negones = None
    B_const = Buf("const")
    S.op("pool", lambda g: g.memset(ident[:], 1.0), writes=[B_const])
    S.op("pool", lambda g: g.affine_select(out=ident[:], in_=ident[:], pattern=[[1, P]], compare_op=ALU.is_equal,
                                           fill=0.0, base=0, channel_multiplier=-1), reads=[B_const], writes=[B_const])
    S.op("pool", lambda g: g.tensor_copy(out=identb[:], in_=ident[:]), reads=[B_const], writes=[B_const])

    def bcast_row(dst_ap, row, c0, c1, Bw):
        S.dma("sp", lambda e: e.dma_start(out=dst_ap, in_=vec[row, c0:c1].partition_broadcast(P)), writes=[Bw])

    if do_p1:
        m1 = C.mark()
        w1T = C.sb("w1T", [P, 8, 1408], BF16)
        w1F = C.sb("w1F", [P, 8, 512], BF16)
        g_bc = C.sb("g_bc", [P, D], F32)
        b_bc = C.sb("b_bc", [P, D], F32)
        oml = C.sb("oml", [P, 256], F32)
        a_tmp = C.sb("a_tmp", [P, 512], F32)
        normg = C.sb("normg", [P, 512], F32)
        U_A = C.sb("U_A", [P, P], F32)
        U_B = C.sb("U_B", [P, P], F32)
        M_A = C.sb("M_A", [P, P], F32)
        M_B = C.sb("M_B", [P, P], F32)
        mask3 = C.sb("mask3", [P, 3, P], F32)
        wgu_pad = C.sb("wgu_pad", [P, P], F32)
        S32 = C.sb("S32", [P, 512], F32)
        Sbf0 = C.sb("Sbf0", [P, 512], BF16)
        Sbf1 = C.sb("Sbf1", [P, 512], BF16)
        B_w1 = Buf("w1")
        B_S32 = [Buf("S32_%d" % h) for h in range(3)]
        B_Sbf0 = [Buf("Sbf0_%d" % h) for h in range(3)]
        B_Sbf1 = [Buf("Sbf1_%d" % h) for h in range(3)]

        S.op("pool", lambda g: g.memset(w1F[:, :, 384:512], 0.0), writes=[B_w1])
        for kc in range(8):
            S.dma("pool", lambda e, kc=kc: e.dma_start(out=w1T[:, kc, :], in_=w1[kc * P:(kc + 1) * P, 0:1408]), writes=[B_w1])
        S.dma("pool", lambda e: e.dma_start(out=w1F[:, :, 0:384], in_=w1[:, 1408:1792].rearrange("(kc p) f -> p kc f", p=P)), writes=[B_w1])
        S.dma("pool", lambda e: e.dma_start(out=w1F[:, :, 384:400], in_=w1[:, 1792:1808].rearrange("(kc p) f -> p kc f", p=P)),
              writes=[B_w1])
        bcast_row(g_bc[:], 0, 0, D, B_const)
        bcast_row(b_bc[:], 1, 0, D, B_const)
        bcast_row(a_tmp[:, 0:256], 6, 0, 256, B_const)
        bcast_row(a_tmp[:, 256:512], 7, 0, 256, B_const)
        bcast_row(normg[:], 8, 0, 512, B_const)
        S.op("dve", lambda v: v.tensor_tensor(out=a_tmp[:, 0:256], in0=a_tmp[:, 0:256], in1=a_tmp[:, 256:512], op=ALU.subtract),
             reads=[B_const], writes=[B_const])
        S.op("act", lambda a: a.activation(out=a_tmp[:, 0:256], in_=a_tmp[:, 0:256], func=AF.Exp), reads=[B_const], writes=[B_const])
        S.op("dve", lambda v: v.tensor_scalar(out=a_tmp[:, 0:256], in0=a_tmp[:, 0:256], scalar1=1.0, scalar2=None, op0=ALU.add),
             reads=[B_const], writes=[B_const])
        S.op("dve", lambda v: v.reciprocal(out=oml[:], in_=a_tmp[:, 0:256]), reads=[B_const], writes=[B_const])
        S.op("pool", lambda g: g.memset(U_A[:], 1.0), writes=[B_const])
        S.op("pool", lambda g: g.affine_select(out=U_A[:], in_=U_A[:], pattern=[[1, P]], compare_op=ALU.is_ge, fill=0.0,
                                               base=0, channel_multiplier=-1), reads=[B_const], writes=[B_const])
        S.op("pool", lambda g: g.memset(U_A[0:64, 64:128], 0.0), reads=[B_const], writes=[B_const])
        S.op("pool", lambda g: g.memset(M_A[:], 1.0), writes=[B_const])
        S.op("pool", lambda g: g.affine_select(out=M_A[:], in_=M_A[:], pattern=[[-1, P]], compare_op=ALU.is_ge, fill=0.0,
                                               base=-1, channel_multiplier=1), reads=[B_const], writes=[B_const])
        S.op("pool", lambda g: g.memset(M_A[64:128, 0:64], 0.0), reads=[B_const], writes=[B_const])
        S.op("pool", lambda g: g.tensor_scalar(out=U_B[:], in0=U_A[:], scalar1=-1.0 / 16.0, scalar2=None, op0=ALU.mult),
             reads=[B_const], writes=[B_const])
        S.op("pool", lambda g: g.tensor_scalar(out=M_B[:], in0=M_A[:], scalar1=-1.0 / 16.0, scalar2=None, op0=ALU.mult),
             reads=[B_const], writes=[B_const])
        for h in range(3):
            S.op("pool", lambda g, h=h: g.tensor_copy(out=mask3[:, h, :], in_=U_A[:]), reads=[B_const], writes=[B_const])
        S.op("pool", lambda g: g.memset(wgu_pad[:], 0.0), writes=[B_const])
        S.dma("sp", lambda e: e.dma_start(out=wgu_pad[0:16, :], in_=wgu_in), reads=[B_const], writes=[B_const])
        S.dma("sp", lambda e: e.dma_start(out=wgu_pad[32:33, :], in_=vec[9:10, 0:128]), reads=[B_const], writes=[B_const])
        S.op("pool", lambda g: g.memset(S32[:], 0.0), writes=B_S32)
        S.op("pool", lambda g: g.memset(Sbf0[:], 0.0), writes=B_Sbf0)
        S.op("pool", lambda g: g.memset(Sbf1[:], 0.0), writes=B_Sbf1)

        NSL = 2
        xt = [C.sb("xt%d" % i, [P, D], F32) for i in range(NSL)]
        xn = [C.sb("xn%d" % i, [P, D], F32) for i in range(NSL)]
        hb = [C.sb("hb%d" % i, [P, D], BF16) for i in range(NSL)]
        hT = [C.sb("hT%d" % i, [P, 8, P], BF16) for i in range(NSL)]
        stt_ = [C.sb("st%d" % i, [P, 12], F32) for i in range(NSL)]
        mv = [C.sb("mv%d" % i, [P, 4], F32) for i in range(NSL)]
        sc = [C.sb("sc%d" % i, [P, 2], F32) for i in range(NSL)]
        k32 = [C.sb("k32_%d" % i, [P, 384], F32) for i in range(NSL)]
        lg = [C.sb("lg_%d" % i, [P, 384], F32) for i in range(NSL)]
        va = [C.sb("va_%d" % i, [P, 512], BF16) for i in range(NSL)]
        vb = [C.sb("vb_%d" % i, [P, 512], BF16) for i in range(NSL)]
        tg = [C.sb("tg_%d" % i, [P, 512], F32) for i in range(NSL)]
        qs = [C.sb("qs_%d" % i, [P, 384], F32) for i in range(NSL)]
        lr_sb = [C.sb("lr_%d" % i, [P, P], F32) for i in range(NSL)]
        ef = [C.sb("ef_%d" % i, [P, 512], F32) for i in range(NSL)]
        tmpA = [C.sb("tmpA_%d" % i, [P, 256], F32) for i in range(NSL)]
        tmpG = [C.sb("tmpG_%d" % i, [P, 512], F32) for i in range(NSL)]
        tmpQ = [C.sb("tmpQ_%d" % i, [P, 256], F32) for i in range(NSL)]
        eg = [C.sb("eg_%d" % i, [P, 512], F32) for i in range(NSL)]
        zg = [C.sb("zg_%d" % i, [P, 512], F32) for i in range(NSL)]
        eq = [C.sb("eq_%d" % i, [P, 256], F32) for i in range(NSL)]
        ebT = [C.sb("ebT_%d" % i, [P, 384], F32) for i in range(NSL)]
        enbT = [C.sb("enbT_%d" % i, [P, 384], F32) for i in range(NSL)]
        edec = [C.sb("edec_%d" % i, [P, 384], F32) for i in range(NSL)]
        kdec = [C.sb("kdec_%d" % i, [P, 384], BF16) for i in range(NSL)]
        ktT = [C.sb("ktT_%d" % i, [P, 384], BF16) for i in range(NSL)]
        qTa = [C.sb("qTa_%d" % i, [P, 3, P], BF16) for i in range(NSL)]
        qTb = [C.sb("qTb_%d" % i, [P, 3, P], BF16) for i in range(NSL)]
        AT = [C.sb("AT_%d" % i, [P, 3, P], BF16) for i in range(NSL)]
        junk = [C.sb("junk_%d" % i, [P, 512], F32) for i in range(NSL)]
        ss = [C.sb("ss_%d" % i, [P, 8], F32) for i in range(NSL)]
        oo = [C.sb("oo_%d" % i, [P, 512], BF16) for i in range(NSL)]
        psA = C.ps("psA", [P, 8, P], BF16)
        psT = [C.ps("psT%d" % i, [P, 512], F32) for i in range(3)]
        psF = C.ps("psF", [P, 512], F32)
        psX = C.ps("psX", [P, 512], F32)
        psY = C.ps("psY", [P, 512], F32)
        psZ = C.ps("psZ", [P, 512], F32)
        Bn = lambda n: [Buf("%s%d" % (n, i)) for i in range(NSL)]
        B_xt, B_xn, B_hb, B_hT, B_st = Bn("xt"), Bn("xn"), Bn("hb"), Bn("hT"), Bn("st")
        B_k32, B_lg, B_va, B_vb, B_tg, B_qs, B_lr = Bn("k32"), Bn("lg"), Bn("va"), Bn("vb"), Bn("tg"), Bn("qs"), Bn("lr")
        B_ef, B_tmpA, B_eg, B_zg, B_eq = Bn("ef"), Bn("tmpA"), Bn("eg"), Bn("zg"), Bn("eq")
        B_tmpG, B_tmpQ = Bn("tmpG"), Bn("tmpQ")
        B_ebT, B_enbT, B_edec, B_kdec, B_ktT = Bn("ebT"), Bn("enbT"), Bn("edec"), Bn("kdec"), Bn("ktT")
        B_qTa, B_qTb, B_AT, B_junk, B_ss, B_oo = Bn("qTa"), Bn("qTb"), Bn("AT"), Bn("junk"), Bn("ss"), Bn("oo")
        B_psA, B_psF, B_psX, B_psY, B_psZ = Buf("psA"), Buf("psF"), Buf("psX"), Buf("psY"), Buf("psZ")
        B_psT = [Buf("psT%d" % i) for i in range(3)]
        for i in range(NSL):
            S.op("pool", lambda g, i=i: g.memset(va[i][:], 0.0), writes=[B_va[i]])
            S.op("pool", lambda g, i=i: g.memset(vb[i][:], 0.0), writes=[B_vb[i]])
            S.op("pool", lambda g, i=i: g.memset(qTa[i][:], 0.0), writes=[B_qTa[i]])
            S.op("pool", lambda g, i=i: g.memset(qTb[i][:], 0.0), writes=[B_qTb[i]])
            S.op("pool", lambda g, i=i: g.memset(lr_sb[i][:], 0.0), writes=[B_lr[i]])
            S.op("pool", lambda g, i=i: g.memset(lr_sb[i][32:33, :], 1.0), reads=[B_lr[i]], writes=[B_lr[i]])

        OC = [(0, 128), (128, 256), (256, 512)]

        def load_x(t):
            s = t % NSL
            S.dma("sp", lambda e: e.dma_start(out=xt[s][:], in_=xb[t * P:(t + 1) * P, :]), writes=[B_xt[s]])

        def stageA(t):
            s = t % NSL
            layernorm_stats(S, xt[s], stt_[s], mv[s], sc[s], B_xt[s], B_st[s], LN_EPS)
            S.op("act", lambda a: a.activation(out=xn[s][:], in_=xt[s][:], func=AF.Identity, scale=sc[s][:, 0:1], bias=sc[s][:, 1:2]),
                 reads=[B_xt[s], B_st[s]], writes=[B_xn[s]])
            S.op("pool", lambda g: g.tensor_tensor(out=xn[s][:], in0=xn[s][:], in1=g_bc[:], op=ALU.mult),
                 reads=[B_xn[s], B_const], writes=[B_xn[s]])
            S.op("pool", lambda g: g.tensor_tensor(out=hb[s][:], in0=xn[s][:], in1=b_bc[:], op=ALU.add),
                 reads=[B_xn[s], B_const], writes=[B_hb[s]])
            for kc in range(8):
                S.op("pe", lambda e, kc=kc: e.transpose(out=psA[:, kc, :], in_=hb[s][:, kc * P:(kc + 1) * P], identity=identb[:]),
                     reads=[B_hb[s], B_const], writes=[B_psA])
            S.op("dve", lambda v: v.tensor_copy(out=hT[s][:], in_=psA[:]), reads=[B_psA], writes=[B_hT[s]])
            tcols = [(0, 384), (384, 896), (896, 1408)]
            for j, (c0, c1) in enumerate(tcols):
                for kc in range(8):
                    S.op("pe", lambda e, kc=kc, j=j, c0=c0, c1=c1: e.matmul(psT[j][:, 0:c1 - c0], lhsT=hT[s][:, kc, :], rhs=w1T[:, kc, c0:c1],
                                                                          start=(kc == 0), stop=(kc == 7)),
                         reads=[B_hT[s], B_w1], writes=[B_psT[j]])
            for j in range(4):
                for kc in range(8):
                    S.op("pe", lambda e, kc=kc, j=j: e.matmul(psF[:, j * P:(j + 1) * P], lhsT=w1F[:, kc, j * P:(j + 1) * P], rhs=hT[s][:, kc, :],
                                                              start=(kc == 0), stop=(kc == 7)),
                         reads=[B_hT[s], B_w1], writes=[B_psF])
            S.op("act", lambda a: a.activation(out=ef[s][:, 0:256], in_=psT[0][:, 0:256], func=AF.Exp), reads=[B_psT[0]], writes=[B_ef[s]])
            S.op("dve", lambda v: v.tensor_copy(out=k32[s][:, 256:384], in_=psT[0][:, 256:384]), reads=[B_psT[0]], writes=[B_k32[s]])
            S.op("act", lambda a: a.activation(out=lr_sb[s][0:16, :], in_=psF[0:16, 384:512], func=AF.Copy), reads=[B_psF], writes=[B_lr[s]])
            S.op("pe", lambda e: e.matmul(psT[0][:, 384:512], lhsT=lr_sb[s][:], rhs=wgu_pad[:], start=True, stop=True),
                 reads=[B_lr[s], B_const], writes=[B_psT[0]])
            recip1p(S, "dve", ef[s][:, 0:256], ef[s][:, 0:256], tmpA[s][:, 0:256], [B_ef[s]], [B_ef[s]], B_tmpA[s])
            S.op("dve", lambda v: v.tensor_tensor(out=k32[s][:, 0:256], in0=ef[s][:, 0:256], in1=oml[:], op=ALU.mult),
                 reads=[B_ef[s], B_const], writes=[B_k32[s]])
            S.op("act", lambda a: a.activation(out=lg[s][:, 0:256], in_=k32[s][:, 0:256], func=AF.Ln, scale=-1.0, bias=1.0),
                 reads=[B_k32[s]], writes=[B_lg[s]])
            S.op("act", lambda a: a.activation(out=ef[s][:, 256:384], in_=psT[0][:, 384:512], func=AF.Exp, scale=-1.0),
                 reads=[B_psT[0]], writes=[B_ef[s]])
            S.op("act", lambda a: a.activation(out=lg[s][:, 256:384], in_=ef[s][:, 256:384], func=AF.Ln, bias=1.0),
                 reads=[B_ef[s]], writes=[B_lg[s]])
            S.op("act", lambda a: a.activation(out=va[s][0:64, :], in_=psT[1][0:64, :], func=AF.Copy), reads=[B_psT[1]], writes=[B_va[s]])
            S.op("act", lambda a: a.activation(out=vb[s][64:128, :], in_=psT[1][64:128, :], func=AF.Copy), reads=[B_psT[1]], writes=[B_vb[s]])
            S.op("act", lambda a: a.activation(out=eg[s][:], in_=psT[2][:], func=AF.Exp, scale=-1.0), reads=[B_psT[2]], writes=[B_eg[s]])
            S.op("dve", lambda v: v.tensor_copy(out=zg[s][:], in_=psT[2][:]), reads=[B_psT[2]], writes=[B_zg[s]])
            recip1p(S, "dve", eg[s][:], eg[s][:], tmpG[s][:], [B_eg[s]], [B_eg[s]], B_tmpG[s])
            S.op("pool", lambda g: g.tensor_tensor(out=zg[s][:], in0=zg[s][:], in1=eg[s][:], op=ALU.mult),
                 reads=[B_zg[s], B_eg[s]], writes=[B_zg[s]])
            S.op("pool", lambda g: g.tensor_tensor(out=tg[s][:], in0=zg[s][:], in1=normg[:], op=ALU.mult),
                 reads=[B_zg[s], B_const], writes=[B_tg[s]])
            S.op("act", lambda a: a.activation(out=eq[s][:], in_=psF[:, 0:256], func=AF.Exp, scale=-1.0), reads=[B_psF], writes=[B_eq[s]])
            recip1p(S, "act", eq[s][:], eq[s][:], tmpQ[s][:], [B_eq[s]], [B_eq[s]], B_tmpQ[s])
            S.op("dve", lambda v: v.tensor_tensor(out=qs[s][:, 0:256], in0=psF[:, 0:256], in1=eq[s][:], op=ALU.mult),
                 reads=[B_psF, B_eq[s]], writes=[B_qs[s]])
            S.op("act", lambda a: a.activation(out=qs[s][:, 256:384], in_=psF[:, 256:384], func=AF.Copy), reads=[B_psF], writes=[B_qs[s]])

        def stageB(t):
            s = t % NSL
            lgs, k32s = lg[s], k32[s]
            for h in range(3):
                Um = U_A if h < 2 else U_B
                S.op("pe", lambda e, h=h, Um=Um: e.matmul(psX[:, h * P:(h + 1) * P], lhsT=lgs[:, h * P:(h + 1) * P], rhs=Um[:], start=True, stop=True),
                     reads=[B_lg[s], B_const], writes=[B_psX])
            S.op("pe", lambda e: e.matmul(psY[:, 0:256], lhsT=M_A[:], rhs=lgs[:, 0:256], start=True, stop=True),
                 reads=[B_lg[s], B_const], writes=[B_psY])
            S.op("pe", lambda e: e.matmul(psY[:, 256:384], lhsT=M_B[:], rhs=lgs[:, 256:384], start=True, stop=True),
                 reads=[B_lg[s], B_const], writes=[B_psY])
            for h in range(3):
                S.op("pe", lambda e, h=h: e.transpose(out=psZ[:, h * P:(h + 1) * P], in_=k32s[:, h * P:(h + 1) * P], identity=ident[:]),
                     reads=[B_k32[s], B_const], writes=[B_psZ])
            S.op("act", lambda a: a.activation(out=ebT[s][:], in_=psX[:, 0:384], func=AF.Exp), reads=[B_psX], writes=[B_ebT[s]])
            S.op("act", lambda a: a.activation(out=enbT[s][:], in_=psX[:, 0:384], func=AF.Exp, scale=-1.0), reads=[B_psX], writes=[B_enbT[s]])
            S.op("act", lambda a: a.activation(out=edec[s][:], in_=psY[:, 0:384], func=AF.Exp), reads=[B_psY], writes=[B_edec[s]])
            S.op("dve", lambda v: v.tensor_tensor(out=kdec[s][:], in0=k32s[:], in1=edec[s][:], op=ALU.mult),
                 reads=[B_k32[s], B_edec[s]], writes=[B_kdec[s]])
            S.op("dve", lambda v: v.tensor_tensor(out=ktT[s][:], in0=psZ[:, 0:384], in1=enbT[s][:], op=ALU.mult),
                 reads=[B_psZ, B_enbT[s]], writes=[B_ktT[s]])
            qs3 = qs[s][:].rearrange("p (h c) -> p h c", h=3)
            eb3 = ebT[s][:].rearrange("p (h c) -> p h c", h=3)
            S.op("dve", lambda v: v.tensor_tensor(out=qTa[s][:, :, 0:64], in0=qs3[:, :, 0:64], in1=eb3[:, :, 0:64], op=ALU.mult),
                 reads=[B_qs[s], B_ebT[s]], writes=[B_qTa[s]])
            S.op("dve", lambda v: v.tensor_tensor(out=qTb[s][:, :, 64:128], in0=qs3[:, :, 64:128], in1=eb3[:, :, 64:128], op=ALU.mult),
                 reads=[B_qs[s], B_ebT[s]], writes=[B_qTb[s]])
            for h in range(3):
                c0, c1 = OC[h]
                S.op("pe", lambda e, h=h, c0=c0, c1=c1: e.matmul(psZ[:, c0:c1], lhsT=kdec[s][:, h * P:(h + 1) * P], rhs=va[s][:, c0:c1], start=True, stop=True),
                     reads=[B_kdec[s], B_va[s]], writes=[B_psZ])
            for h in range(3):
                S.op("pe", lambda e, h=h: e.matmul(psX[:, h * P:(h + 1) * P], lhsT=ktT[s][:, h * P:(h + 1) * P], rhs=qTa[s][:, h, :], start=True, stop=False),
                     reads=[B_ktT[s], B_qTa[s]], writes=[B_psX])
                S.op("pe", lambda e, h=h: e.matmul(psX[:, h * P:(h + 1) * P], lhsT=ktT[s][:, h * P:(h + 1) * P], rhs=qTb[s][:, h, :], start=False, stop=True),
                     reads=[B_ktT[s], B_qTb[s]], writes=[B_psX])
            for h in range(3):
                c0, c1 = OC[h]
                S.op("dve", lambda v, h=h, c0=c0, c1=c1: v.scalar_tensor_tensor(out=S32[:, c0:c1], in0=S32[:, c0:c1], scalar=ebT[s][:, h * P + 63:h * P + 64],
                                                                              in1=psZ[:, c0:c1], op0=ALU.mult, op1=ALU.add),
                     reads=[B_S32[h], B_ebT[s], B_psZ], writes=[B_S32[h]])
                S.op("act", lambda a, c0=c0, c1=c1: a.activation(out=Sbf1[:, c0:c1], in_=S32[:, c0:c1], func=AF.Copy),
                     reads=[B_S32[h]], writes=[B_Sbf1[h]])
            S.op("dve", lambda v: v.tensor_tensor(out=AT[s][:], in0=psX[:, 0:384].rearrange("p (h c) -> p h c", h=3), in1=mask3[:], op=ALU.mult),
                 reads=[B_psX, B_const], writes=[B_AT[s]])
            for h in range(3):
                c0, c1 = OC[h]
                S.op("pe", lambda e, h=h, c0=c0, c1=c1: e.matmul(psY[:, c0:c1], lhsT=AT[s][:, h, :], rhs=va[s][:, c0:c1], start=True, stop=False),
                     reads=[B_AT[s], B_va[s]], writes=[B_psY])
                S.op("pe", lambda e, h=h, c0=c0, c1=c1: e.matmul(psY[:, c0:c1], lhsT=AT[s][:, h, :], rhs=vb[s][:, c0:c1], start=False, stop=False),
                     reads=[B_AT[s], B_vb[s]], writes=[B_psY])
                S.op("pe", lambda e, h=h, c0=c0, c1=c1: e.matmul(psY[:, c0:c1], lhsT=qTa[s][:, h, :], rhs=Sbf0[:, c0:c1], start=False, stop=False),
                     reads=[B_qTa[s], B_Sbf0[h]], writes=[B_psY])
                S.op("pe", lambda e, h=h, c0=c0, c1=c1: e.matmul(psY[:, c0:c1], lhsT=qTb[s][:, h, :], rhs=Sbf1[:, c0:c1], start=False, stop=True),
                     reads=[B_qTb[s], B_Sbf1[h]], writes=[B_psY])
            for h in range(3):
                c0, c1 = OC[h]
                S.op("pe", lambda e, h=h, c0=c0, c1=c1: e.matmul(psX[:, c0:c1], lhsT=kdec[s][:, h * P:(h + 1) * P], rhs=vb[s][:, c0:c1], start=True, stop=True),
                     reads=[B_kdec[s], B_vb[s]], writes=[B_psX])
            for h in range(3):
                c0, c1 = OC[h]
                S.op("dve", lambda v, h=h, c0=c0, c1=c1: v.scalar_tensor_tensor(out=S32[:, c0:c1], in0=S32[:, c0:c1], scalar=ebT[s][:, h * P + 127:h * P + 128],
                                                                              in1=psX[:, c0:c1], op0=ALU.mult, op1=ALU.add),
                     reads=[B_S32[h], B_ebT[s], B_psX], writes=[B_S32[h]])
                S.op("act", lambda a, c0=c0, c1=c1: a.activation(out=Sbf0[:, c0:c1], in_=S32[:, c0:c1], func=AF.Copy),
                     reads=[B_S32[h]], writes=[B_Sbf0[h]])
            for h in range(3):
                c0, c1 = OC[h]
                S.op("act", lambda a, h=h, c0=c0, c1=c1: a.activation(out=junk[s][:, c0:c1], in_=psY[:, c0:c1], func=AF.Square, accum_out=ss[s][:, h:h + 1]),
                     reads=[B_psY], writes=[B_junk[s], B_ss[s]])
            S.op("act", lambda a: a.activation(out=ss[s][:, 4:6], in_=ss[s][:, 0:2], func=AF.Ln, scale=1.0 / 128.0, bias=RMS_EPS * 128.0),
                 reads=[B_ss[s]], writes=[B_ss[s]])
            S.op("act", lambda a: a.activation(out=ss[s][:, 6:7], in_=ss[s][:, 2:3], func=AF.Ln, scale=1.0 / 256.0, bias=RMS_EPS * 128.0),
                 reads=[B_ss[s]], writes=[B_ss[s]])
            S.op("act", lambda a: a.activation(out=ss[s][:, 4:7], in_=ss[s][:, 4:7], func=AF.Exp, scale=-0.5), reads=[B_ss[s]], writes=[B_ss[s]])
            for h in range(3):
                c0, c1 = OC[h]
                S.op("dve", lambda v, h=h, c0=c0, c1=c1: v.scalar_tensor_tensor(out=oo[s][:, c0:c1], in0=psY[:, c0:c1], scalar=ss[s][:, 4 + h:5 + h],
                                                                              in1=tg[s][:, c0:c1], op0=ALU.mult, op1=ALU.mult),
                     reads=[B_psY, B_ss[s], B_tg[s]], writes=[B_oo[s]])
            S.dma("sp", lambda e: e.dma_start(out=oex[t * P:(t + 1) * P, :], in_=oo[s][:]), reads=[B_oo[s]], writes=[B_oex])

        KP1 = os.environ.get("KP1", "full")
        if KP1 != "const":
            load_x(0)
            if n_tiles_p1 > 1:
                load_x(1)
            stageA(0)
        for t in range(n_tiles_p1 if KP1 != "const" else 0):
            if t + 1 < n_tiles_p1:
                stageA(t + 1)
            if KP1 == "full":
                stageB(t)
            if t + 2 < n_tiles_p1:
                load_x(t + 2)
        if debug:
            S.dma("sp", lambda e: e.dma_start(out=dbg_o[0:n_tiles_p1 * P, :], in_=oex[0:n_tiles_p1 * P, :]), reads=[B_oex], writes=[Buf("dbg")])
        S.barrier()
        C.release(m1)


    if do_p2:
        oidx_in = din("oidx", [P, n_tiles_p2 * 4], I32)
        S.cc(lambda g: g.collective_compute("AllGather", ALU.bypass, replica_groups=[list(range(NCORES))],
                                            ins=[oex.opt()], outs=[oga.opt()]), reads=[B_oex], writes=[B_oga])
        NT2 = n_tiles_p2
        REG_OGA = nc.gpsimd.to_reg(NCORES * SEQ - 1)
        REG_SLOT = nc.gpsimd.to_reg(NSLOT - 1)
        xg = nc.dram_tensor("xg", [NSLOT, D], BF16).ap()
        yg = nc.dram_tensor("yg", [NSLOT, D], F32).ap()
        rbuf = nc.dram_tensor("rbuf", [TSEG, D], F32).ap()
        B_xg, B_yg, B_rbuf = Buf("xg"), Buf("yg"), Buf("rbuf")
        mP = C.mark()
        slots_all = C.sb("slots_all", [P, NT2, 4], I32)
        wts_all = C.sb("wts_all", [P, NT2, 4], F32)
        WT_all = C.sb("WT_all", [P, NT2, P], F32)
        gbc2 = C.sb("gbc2", [P, D], F32)
        bbc2 = C.sb("bbc2", [P, D], F32)
        B_slots, B_wts, B_WT = Buf("slots"), Buf("wts"), Buf("WT")
        S.op("pool", lambda g: g.memset(WT_all[:], 0.0), writes=[B_WT])
        bcast_row(gbc2[:], 4, 0, D, B_const)
        bcast_row(bbc2[:], 5, 0, D, B_const)

        mA = C.mark()
        wgT = C.sb("wgT", [P, 8, 2048], BF16)
        wpaT = C.sb("wpaT", [P, 8, D], BF16)
        wpbT = C.sb("wpbT", [P, 8, D], BF16)
        woT = C.sb("woT", [P, 8, D], BF16)
        wplgT = C.sb("wplgT", [P, 8, D], BF16)
        wplpT = C.sb("wplpT", [P, 2, D], BF16)
        wr32 = C.sb("wr32", [P, 8, NEXP], F32)
        B_w2 = Buf("w2")
        for kc in range(8):
            S.dma("pool", lambda e, kc=kc: e.dma_start(out=wgT[:, kc, :], in_=wgate[kc * P:(kc + 1) * P, :]), writes=[B_w2])
        for (dst, src) in ((wpaT, wpa), (wpbT, wpb), (woT, wout), (wplgT, wplg)):
            S.dma("pool", lambda e, dst=dst, src=src: e.dma_start(out=dst[:], in_=src.rearrange("(kc p) f -> p kc f", p=P)), writes=[B_w2])
        S.dma("pool", lambda e: e.dma_start(out=wplpT[:], in_=wplp.rearrange("(kc p) f -> p kc f", p=P)), writes=[B_w2])
        S.dma("sp", lambda e: e.dma_start(out=wr32[:], in_=wr.rearrange("(kc p) f -> p kc f", p=P)), writes=[B_w2])
        gbc0 = C.sb("gbc0", [P, D], F32); bbc0 = C.sb("bbc0", [P, D], F32)
        gbc1 = C.sb("gbc1", [P, D], F32); bbc1 = C.sb("bbc1", [P, D], F32)
        bcast_row(gbc0[:], 0, 0, D, B_const); bcast_row(bbc0[:], 1, 0, D, B_const)
        bcast_row(gbc1[:], 2, 0, D, B_const); bcast_row(bbc1[:], 3, 0, D, B_const)
        ones_pad = C.sb("ones_pad", [P, P], F32)
        br_pad = C.sb("br_pad", [P, NEXP], F32)
        Lstrict = C.sb("Lstrict", [P, P], F32)
        ones128 = C.sb("ones128", [P, P], F32)
        ebase = C.sb("ebase", [P, NEXP], F32)
        offs_bc = C.sb("offs_bc", [P, NEXP], F32)
        oidx = C.sb("oidx_sb", [P, NT2 * 4], I32)
        B_offs = Buf("offs")
        S.op("pool", lambda g: g.memset(ones_pad[:], 0.0), writes=[B_const])
        S.op("pool", lambda g: g.memset(ones_pad[0:1, :], 1.0), reads=[B_const], writes=[B_const])
        S.op("pool", lambda g: g.memset(br_pad[:], 0.0), writes=[B_const])
        S.dma("sp", lambda e: e.dma_start(out=br_pad[0:1, :], in_=vec[10:11, 0:NEXP]), reads=[B_const], writes=[B_const])
        S.op("pool", lambda g: g.memset(Lstrict[:], 1.0), writes=[B_const])
        S.op("pool", lambda g: g.affine_select(out=Lstrict[:], in_=Lstrict[:], pattern=[[1, P]], compare_op=ALU.is_ge, fill=0.0,
                                               base=-1, channel_multiplier=-1), reads=[B_const], writes=[B_const])
        S.op("pool", lambda g: g.memset(ones128[:], 1.0), writes=[B_const])
        S.op("pool", lambda g: g.iota(ebase[:], pattern=[[CAP, NEXP]], base=0, channel_multiplier=0, allow_small_or_imprecise_dtypes=True),
             writes=[B_const])
        S.op("pool", lambda g: g.memset(offs_bc[:], 0.0), writes=[B_offs])
        S.dma("sp", lambda e: e.dma_start(out=oidx[:], in_=oidx_in), writes=[B_const])

        xt2 = C.sb("xt2", [P, D], F32); h32 = C.sb("h32", [P, D], F32); hb2 = C.sb("hb2", [P, D], BF16)
        hT2 = C.sb("hT2", [P, 8, P], BF16)
        st2 = C.sb("st2", [P, 12], F32); mv2 = C.sb("mv2", [P, 4], F32); sc2 = C.sb("sc2", [P, 2], F32)
        otok = C.sb("otok", [P, 2048], BF16); oT = C.sb("oT", [P, 16, P], BF16)
        ea = C.sb("ea", [P, 512], F32); eb = C.sb("eb", [P, 512], F32); tm = C.sb("tm", [P, 512], F32)
        m1 = ea; m2 = eb
        mT = C.sb("mT", [P, 8, P], BF16)
        r1 = xt2; h2 = C.sb("h2", [P, D], F32); h2b = C.sb("h2b", [P, D], BF16)
        h2T32 = C.sb("h2T32", [P, 8, P], F32); h2Tb = C.sb("h2Tb", [P, 8, P], BF16)
        lgt = C.sb("lgt", [P, NEXP], F32); top8 = C.sb("top8", [P, 8], F32); sm = C.sb("sm", [P, 8], F32)
        ew = C.sb("ew", [P, 4], F32); ohk = C.sb("ohk", [P, 4, NEXP], F32); msk = C.sb("msk", [P, NEXP], F32)
        pos = C.sb("pos", [P, NEXP], F32); tmp32 = C.sb("tmp32", [P, NEXP], F32); slotf = C.sb("slotf", [P, 4], F32)
        Wg = C.sb("Wg", [P, NEXP], F32)
        pt = C.sb("pt", [P, 256], F32); pb = C.sb("pb", [P, 256], BF16); pT = C.sb("pT", [P, 2, P], BF16)
        eg2 = C.sb("eg2", [P, D], F32); tg2 = xt2; r2 = h32
        psB = [C.ps("psB%d" % i, [P, 8, P], BF16) for i in range(2)]
        psM = [C.ps("psM%d" % i, [P, 512], F32) for i in range(6)]
        B_ = {n: Buf(n) for n in ["xt2", "h32", "hb2", "hT2", "st2", "otok", "oT", "ea", "eb", "tm", "m1", "m2", "mT", "r1", "h2", "h2b",
                                  "h2T32", "h2Tb", "lgt", "top8", "sm", "ew", "ohk", "msk", "pos", "tmp32", "slotf", "Wg", "pt", "pb",
                                  "pT", "eg2", "tg2", "r2", "psB0", "psB1", "psM0", "psM1", "psM2", "psM3", "psM4", "psM5"]}
        B_["m1"] = B_["ea"]; B_["m2"] = B_["eb"]; B_["r1"] = B_["xt2"]; B_["tg2"] = B_["xt2"]; B_["r2"] = B_["h32"]
        B_psB = [B_["psB0"], B_["psB1"]]
        B_psM = [B_["psM%d" % i] for i in range(6)]

        def ln_apply(x_ap, Bx, stt, mvv, scc, Bst, gb, bb, out_ap, Bout, eps):
            layernorm_stats(S, x_ap, stt, mvv, scc, Bx, Bst, eps)
            S.op("act", lambda a: a.activation(out=out_ap, in_=x_ap, func=AF.Identity, scale=scc[:, 0:1], bias=scc[:, 1:2]),
                 reads=[Bx, Bst], writes=[Bout])
            S.op("pool", lambda g: g.tensor_tensor(out=out_ap, in0=out_ap, in1=gb[:], op=ALU.mult), reads=[Bout, B_const], writes=[Bout])
            S.op("pool", lambda g: g.tensor_tensor(out=out_ap, in0=out_ap, in1=bb[:], op=ALU.add), reads=[Bout, B_const], writes=[Bout])

        for i in range(NT2):
            S.dma("sp", lambda e, i=i: e.dma_start(out=xt2[:], in_=xseg[i * P:(i + 1) * P, :]), writes=[B_["xt2"]])
            for hg in range(4):
                S.dma("pool", lambda e, i=i, hg=hg: e.indirect_dma_start(
                    out=otok[:, hg * 512:(hg + 1) * 512], out_offset=None, in_=oga[:, :],
                    in_offset=bass.IndirectOffsetOnAxis(ap=oidx[:, i * 4 + hg:i * 4 + hg + 1], axis=0),
                    bounds_check=REG_OGA, oob_is_err=False), reads=[B_oga, B_const], writes=[B_["otok"]])
            S.dma("sp", lambda e, i=i: e.dma_start(out=pt[:], in_=pseg[i * P:(i + 1) * P, :]), writes=[B_["pt"]])
            ln_apply(xt2[:], B_["xt2"], st2, mv2, sc2, B_["st2"], gbc0, bbc0, h32[:], B_["h32"], LN_EPS)
            S.op("pool", lambda g: g.tensor_copy(out=hb2[:], in_=h32[:]), reads=[B_["h32"]], writes=[B_["hb2"]])
            for kc in range(8):
                S.op("pe", lambda e, kc=kc: e.transpose(out=psB[0][:, kc, :], in_=hb2[:, kc * P:(kc + 1) * P], identity=identb[:]),
                     reads=[B_["hb2"], B_const], writes=[B_psB[0]])
            S.op("dve", lambda v: v.tensor_copy(out=hT2[:], in_=psB[0][:]), reads=[B_psB[0]], writes=[B_["hT2"]])
            for half in range(2):
                for c in range(8):
                    S.op("pe", lambda e, c=c, half=half: e.transpose(out=psB[half][:, c, :], in_=otok[:, (half * 8 + c) * P:(half * 8 + c + 1) * P],
                                                                    identity=identb[:]), reads=[B_["otok"], B_const], writes=[B_psB[half]])
                S.op("act" if half else "dve", (lambda a, half=half: a.activation(out=oT[:, half * 8:(half + 1) * 8, :], in_=psB[half][:], func=AF.Copy)) if half else
                     (lambda v, half=half: v.tensor_copy(out=oT[:, half * 8:(half + 1) * 8, :], in_=psB[half][:])),
                     reads=[B_psB[half]], writes=[B_["oT"]])
            for half in range(2):
                for dcl in range(4):
                    dc = half * 4 + dcl
                    cs = slice(dc * P, (dc + 1) * P)
                    ps_sl = slice(dcl * P, (dcl + 1) * P)
                    n = 0
                    for hg in range(4):
                        for j in range(2):
                            S.op("pe", lambda e, hg=hg, j=j, cs=cs, ps_sl=ps_sl, n=n: e.matmul(psM[0][:, ps_sl], lhsT=wpaT[:, 2 * hg + j, cs], rhs=oT[:, hg * 4 + j, :],
                                                                                                 start=(n == 0), stop=(n == 7)),
                                 reads=[B_w2, B_["oT"]], writes=[B_psM[0]])
                            n += 1
                    n = 0
                    for hg in range(4):
                        for j in range(2):
                            S.op("pe", lambda e, hg=hg, j=j, cs=cs, ps_sl=ps_sl, n=n: e.matmul(psM[1][:, ps_sl], lhsT=wpbT[:, 2 * hg + j, cs], rhs=oT[:, hg * 4 + 2 + j, :],
                                                                                                 start=(n == 0), stop=(n == 7)),
                                 reads=[B_w2, B_["oT"]], writes=[B_psM[1]])
                            n += 1
                    for gi in range(2):
                        for kc in range(8):
                            S.op("pe", lambda e, kc=kc, gi=gi, dc=dc, ps_sl=ps_sl: e.matmul(psM[2 + gi][:, ps_sl], lhsT=wgT[:, kc, gi * D + dc * P:gi * D + (dc + 1) * P],
                                                                                            rhs=hT2[:, kc, :], start=(kc == 0), stop=(kc == 7)),
                                 reads=[B_w2, B_["hT2"]], writes=[B_psM[2 + gi]])
                S.op("act", lambda a: a.activation(out=ea[:], in_=psM[2][:], func=AF.Exp, scale=-1.0), reads=[B_psM[2]], writes=[B_["ea"]])
                S.op("act", lambda a: a.activation(out=eb[:], in_=psM[3][:], func=AF.Exp, scale=-1.0), reads=[B_psM[3]], writes=[B_["eb"]])
                recip1p(S, "dve", ea[:], ea[:], tm[:], [B_["ea"]], [B_["ea"]], B_["tm"])
                recip1p(S, "dve", eb[:], eb[:], tm[:], [B_["eb"]], [B_["eb"]], B_["tm"])
                S.op("dve", lambda v: v.tensor_tensor(out=m1[:], in0=psM[0][:], in1=ea[:], op=ALU.mult), reads=[B_psM[0], B_["ea"]], writes=[B_["m1"]])
                S.op("dve", lambda v: v.tensor_tensor(out=m2[:], in0=psM[1][:], in1=eb[:], op=ALU.mult), reads=[B_psM[1], B_["eb"]], writes=[B_["m2"]])
                S.op("pool", lambda g, half=half: g.tensor_tensor(out=mT[:, half * 4:(half + 1) * 4, :].rearrange("p a b -> p (a b)"), in0=m1[:], in1=m2[:], op=ALU.add),
                     reads=[B_["m1"], B_["m2"]], writes=[B_["mT"]])
            for half in range(2):
                for kc in range(8):
                    S.op("pe", lambda e, kc=kc, half=half: e.matmul(psM[4 + half][:], lhsT=mT[:, kc, :], rhs=woT[:, kc, half * 512:(half + 1) * 512],
                                                                    start=(kc == 0), stop=(kc == 7)), reads=[B_["mT"], B_w2], writes=[B_psM[4 + half]])
                S.op("dve", lambda v, half=half: v.scalar_tensor_tensor(out=r1[:, half * 512:(half + 1) * 512], in0=h32[:, half * 512:(half + 1) * 512], scalar=DN_ALPHA,
                                                                        in1=psM[4 + half][:], op0=ALU.mult, op1=ALU.add),
                     reads=[B_["h32"], B_psM[4 + half]], writes=[B_["r1"]])
            ln_apply(r1[:], B_["r1"], st2, mv2, sc2, B_["st2"], gbc1, bbc1, h2[:], B_["h2"], LN_EPS)
            S.op("pool", lambda g: g.tensor_copy(out=h2b[:], in_=h2[:]), reads=[B_["h2"]], writes=[B_["h2b"]])
            for half in range(2):
                for c in range(4):
                    kc = half * 4 + c
                    S.op("pe", lambda e, kc=kc, c=c, half=half: e.transpose(out=psM[half][:, c * P:(c + 1) * P], in_=h2[:, kc * P:(kc + 1) * P], identity=ident[:]),
                         reads=[B_["h2"], B_const], writes=[B_psM[half]])
                S.op("act", lambda a, half=half: a.activation(out=h2T32[:, half * 4:(half + 1) * 4, :].rearrange("p a b -> p (a b)"), in_=psM[half][:], func=AF.Copy),
                     reads=[B_psM[half]], writes=[B_["h2T32"]])
                S.op("dve", lambda v, half=half: v.tensor_copy(out=h2Tb[:, half * 4:(half + 1) * 4, :].rearrange("p a b -> p (a b)"), in_=psM[half][:]),
                     reads=[B_psM[half]], writes=[B_["h2Tb"]])
            for kc in range(8):
                S.op("pe", lambda e, kc=kc: e.matmul(psM[2][:, 0:NEXP], lhsT=h2T32[:, kc, :], rhs=wr32[:, kc, :], start=(kc == 0), stop=False),
                     reads=[B_["h2T32"], B_w2], writes=[B_psM[2]])
            S.op("pe", lambda e: e.matmul(psM[2][:, 0:NEXP], lhsT=ones_pad[:], rhs=br_pad[:], start=False, stop=True), reads=[B_const], writes=[B_psM[2]])
            S.op("dve", lambda v: v.tensor_copy(out=lgt[:], in_=psM[2][:, 0:NEXP]), reads=[B_psM[2]], writes=[B_["lgt"]])
            S.op("dve", lambda v: v.max(out=top8[:], in_=lgt[:]), reads=[B_["lgt"]], writes=[B_["top8"]])
            S.op("dve", lambda v: v.tensor_scalar(out=sm[:, 0:1], in0=top8[:, 0:1], scalar1=-1.0, scalar2=None, op0=ALU.mult), reads=[B_["top8"]], writes=[B_["sm"]])
            S.op("act", lambda a: a.activation(out=ew[:], in_=top8[:, 0:4], func=AF.Exp, bias=sm[:, 0:1], accum_out=sm[:, 1:2]),
                 reads=[B_["top8"], B_["sm"]], writes=[B_["ew"], B_["sm"]])
            S.op("dve", lambda v: v.reciprocal(out=sm[:, 2:3], in_=sm[:, 1:2]), reads=[B_["sm"]], writes=[B_["sm"]])
            S.op("dve", lambda v, i=i: v.tensor_scalar(out=wts_all[:, i, :], in0=ew[:], scalar1=sm[:, 2:3], scalar2=None, op0=ALU.mult),
                 reads=[B_["ew"], B_["sm"]], writes=[B_wts])
            for k in range(4):
                S.op("dve", lambda v, k=k: v.tensor_scalar(out=ohk[:, k, :], in0=lgt[:], scalar1=top8[:, k:k + 1], scalar2=None, op0=ALU.is_equal),
                     reads=[B_["lgt"], B_["top8"]], writes=[B_["ohk"]])
            S.op("dve", lambda v: v.tensor_scalar(out=msk[:], in0=lgt[:], scalar1=top8[:, 3:4], scalar2=None, op0=ALU.is_ge),
                 reads=[B_["lgt"], B_["top8"]], writes=[B_["msk"]])
            S.op("pe", lambda e: e.matmul(psM[3][:, 0:NEXP], lhsT=Lstrict[:], rhs=msk[:], start=True, stop=True), reads=[B_["msk"], B_const], writes=[B_psM[3]])
            S.op("pe", lambda e: e.matmul(psM[3][:, 64:64 + NEXP], lhsT=ones128[:], rhs=msk[:], start=True, stop=True), reads=[B_["msk"], B_const], writes=[B_psM[3]])
            S.op("dve", lambda v: v.tensor_tensor(out=pos[:], in0=psM[3][:, 0:NEXP], in1=offs_bc[:], op=ALU.add), reads=[B_psM[3], B_offs], writes=[B_["pos"]])
            S.op("dve", lambda v: v.tensor_tensor(out=offs_bc[:], in0=psM[3][:, 64:64 + NEXP], in1=offs_bc[:], op=ALU.add), reads=[B_psM[3], B_offs], writes=[B_offs])
            S.op("dve", lambda v: v.scalar_tensor_tensor(out=pos[:], in0=pos[:], scalar=float(CAP - 1), in1=ebase[:], op0=ALU.min, op1=ALU.add),
                 reads=[B_["pos"], B_const], writes=[B_["pos"]])
            for k in range(4):
                S.op("dve", lambda v, k=k: v.tensor_tensor(out=tmp32[:], in0=ohk[:, k, :], in1=pos[:], op=ALU.mult), reads=[B_["ohk"], B_["pos"]], writes=[B_["tmp32"]])
                S.op("dve", lambda v, k=k: v.reduce_sum(out=slotf[:, k:k + 1], in_=tmp32[:], axis=AX.X), reads=[B_["tmp32"]], writes=[B_["slotf"]])
            S.op("dve", lambda v, i=i: v.tensor_copy(out=slots_all[:, i, :], in_=slotf[:]), reads=[B_["slotf"]], writes=[B_slots])
            S.op("dve", lambda v, i=i: v.tensor_scalar(out=Wg[:], in0=ohk[:, 0, :], scalar1=wts_all[:, i, 0:1], scalar2=None, op0=ALU.mult),
                 reads=[B_["ohk"], B_wts], writes=[B_["Wg"]])
            for k in range(1, 4):
                S.op("dve", lambda v, i=i, k=k: v.scalar_tensor_tensor(out=Wg[:], in0=ohk[:, k, :], scalar=wts_all[:, i, k:k + 1], in1=Wg[:], op0=ALU.mult, op1=ALU.add),
                     reads=[B_["ohk"], B_wts, B_["Wg"]], writes=[B_["Wg"]])
            S.op("pe", lambda e: e.transpose(out=psM[3][0:NEXP, 128:256], in_=Wg[:], identity=ident[:]), reads=[B_["Wg"], B_const], writes=[B_psM[3]])
            S.op("act", lambda a, i=i: a.activation(out=WT_all[0:NEXP, i, :], in_=psM[3][0:NEXP, 128:256], func=AF.Copy), reads=[B_psM[3]], writes=[B_WT])
            for k in range(4):
                S.dma("pool", lambda e, i=i, k=k: e.indirect_dma_start(out=xg[:, :], out_offset=bass.IndirectOffsetOnAxis(ap=slots_all[:, i, k:k + 1], axis=0),
                                                                      in_=h2b[:, :], in_offset=None, bounds_check=REG_SLOT, oob_is_err=False),
                      reads=[B_["h2b"], B_slots], writes=[B_xg])
            S.op("pool", lambda g: g.tensor_copy(out=pb[:], in_=pt[:]), reads=[B_["pt"]], writes=[B_["pb"]])
            for c in range(2):
                S.op("pe", lambda e, c=c: e.transpose(out=psB[1][:, c, :], in_=pb[:, c * P:(c + 1) * P], identity=identb[:]), reads=[B_["pb"], B_const], writes=[B_psB[1]])
            S.op("dve", lambda v: v.tensor_copy(out=pT[:], in_=psB[1][:, 0:2, :]), reads=[B_psB[1]], writes=[B_["pT"]])
            for half in range(2):
                hs = slice(half * 512, (half + 1) * 512)
                for kc in range(8):
                    S.op("pe", lambda e, kc=kc, half=half, hs=hs: e.matmul(psM[half][:], lhsT=h2Tb[:, kc, :], rhs=wplgT[:, kc, hs], start=(kc == 0), stop=(kc == 7)),
                         reads=[B_["h2Tb"], B_w2], writes=[B_psM[half]])
                for kc in range(2):
                    S.op("pe", lambda e, kc=kc, half=half, hs=hs: e.matmul(psM[4 + half][:], lhsT=pT[:, kc, :], rhs=wplpT[:, kc, hs], start=(kc == 0), stop=(kc == 1)),
                         reads=[B_["pT"], B_w2], writes=[B_psM[4 + half]])
                S.op("act", lambda a, half=half, hs=hs: a.activation(out=eg2[:, hs], in_=psM[half][:], func=AF.Exp, scale=-1.0), reads=[B_psM[half]], writes=[B_["eg2"]])
            recip1p(S, "dve", eg2[:], eg2[:], tg2[:], [B_["eg2"]], [B_["eg2"]], B_["tg2"])
            for half in range(2):
                hs = slice(half * 512, (half + 1) * 512)
                S.op("dve", lambda v, half=half, hs=hs: v.tensor_tensor(out=tg2[:, hs], in0=psM[4 + half][:], in1=eg2[:, hs], op=ALU.mult),
                     reads=[B_psM[4 + half], B_["eg2"]], writes=[B_["tg2"]])
            S.op("dve", lambda v: v.scalar_tensor_tensor(out=r2[:], in0=h2[:], scalar=DN_ALPHA, in1=tg2[:], op0=ALU.mult, op1=ALU.add),
                 reads=[B_["h2"], B_["tg2"]], writes=[B_["r2"]])
            S.dma("sp", lambda e, i=i: e.dma_start(out=rbuf[i * P:(i + 1) * P, :], in_=r2[:]), reads=[B_["r2"]], writes=[B_rbuf])
        S.barrier()
        C.release(mA)

        mB = C.mark()
        RT = CAP // P
        NC2 = CAP // 2
        Wgu = [C.sb("Wgu%d" % i, [P, 8, 2 * D], BF16) for i in range(2)]
        Wd = [C.sb("Wd%d" % i, [P, 8, D], BF16) for i in range(2)]
        B_Wgu = [Buf("Wgu0"), Buf("Wgu1")]
        B_Wd = [Buf("Wd0"), Buf("Wd1")]
        bg_in = C.sb("bg_in", [NEXP, 2 * D], F32)
        bguT = C.sb("bguT", [P, 16, NEXP], F32)
        Xr = [C.sb("Xr%d" % i, [P, RT, D], BF16) for i in range(2)]
        XT = [C.sb("XT%d" % i, [P, 8, CAP], BF16) for i in range(2)]
        gsb = [C.sb("gsb%d" % i, [P, NC2], F32) for i in range(2)]
        usb = [C.sb("usb%d" % i, [P, NC2], F32) for i in range(2)]
        esb = [C.sb("esb%d" % i, [P, NC2], F32) for i in range(2)]
        tsb = [C.sb("tsb%d" % i, [P, NC2], F32) for i in range(2)]
        actT = C.sb("actT", [P, 8, CAP], BF16)
        yrow = [C.sb("yrow%d" % i, [P, D], F32) for i in range(2)]
        psBx = [C.ps("psBx%d" % i, [P, 8, P], BF16) for i in range(2)]
        psG = [C.ps("psG%d" % i, [P, 512], F32) for i in range(2)]
        psU = [C.ps("psU%d" % i, [P, 512], F32) for i in range(2)]
        psD = [C.ps("psD%d" % i, [P, 512], F32) for i in range(2)]
        Bb = {n: Buf(n) for n in ["bg_in", "bguT", "Xr0", "Xr1", "XT0", "XT1", "gsb0", "gsb1", "usb0", "usb1", "esb0", "esb1", "tsb0", "tsb1", "actT",
                                  "yrow0", "yrow1", "psBx0", "psBx1", "psG0", "psG1", "psU0", "psU1", "psD0", "psD1"]}

        def load_w(e_):
            s_ = e_ % 2
            for kc in range(8):
                S.dma("pool", lambda e, kc=kc: e.dma_start(out=Wgu[s_][:, kc, :], in_=wgup[e_, kc * P:(kc + 1) * P, :]), writes=[B_Wgu[s_]])
            for kc in range(8):
                S.dma("pool", lambda e, kc=kc: e.dma_start(out=Wd[s_][:, kc, :], in_=wdn[e_, kc * P:(kc + 1) * P, :]), writes=[B_Wd[s_]])

        load_w(0)
        S.dma("sp", lambda e: e.dma_start(out=bg_in[:], in_=bgup), writes=[Bb["bg_in"]])
        for fc in range(16):
            S.op("pe", lambda e, fc=fc: e.transpose(out=psD[fc // 8][:, (fc % 8) * NEXP:(fc % 8 + 1) * NEXP],
                                                    in_=bg_in[:, fc * P:(fc + 1) * P], identity=ident[0:NEXP, 0:NEXP]),
                 reads=[Bb["bg_in"], B_const], writes=[Bb["psD%d" % (fc // 8)]])
        for hh in range(2):
            S.op("dve", lambda v, hh=hh: v.tensor_copy(out=bguT[:, hh * 8:(hh + 1) * 8, :].rearrange("p a b -> p (a b)"), in_=psD[hh][:, 0:8 * NEXP]),
                 reads=[Bb["psD%d" % hh]], writes=[Bb["bguT"]])
        n_exp = int(os.environ.get("KNEXP", NEXP))

        def stageX(e_):
            x_ = e_ % 2
            S.dma("sp", lambda e: e.dma_start(out=Xr[x_][:], in_=xg[e_ * CAP:(e_ + 1) * CAP, :].rearrange("(rt p) f -> p rt f", p=P)),
                  reads=[B_xg], writes=[Bb["Xr%d" % x_]])
            for rt in range(RT):
                pb_ = rt % 2
                for kc in range(8):
                    S.op("pe", lambda e, kc=kc, rt=rt, pb_=pb_: e.transpose(out=psBx[pb_][:, kc, :], in_=Xr[x_][:, rt, kc * P:(kc + 1) * P], identity=identb[:]),
                         reads=[Bb["Xr%d" % x_], B_const], writes=[Bb["psBx%d" % pb_]])
                if rt % 2:
                    S.op("dve", lambda v, rt=rt, pb_=pb_: v.tensor_copy(out=XT[x_][:, :, rt * P:(rt + 1) * P], in_=psBx[pb_][:]),
                         reads=[Bb["psBx%d" % pb_]], writes=[Bb["XT%d" % x_]])
                else:
                    S.op("act", lambda a, rt=rt, pb_=pb_: a.activation(out=XT[x_][:, :, rt * P:(rt + 1) * P], in_=psBx[pb_][:], func=AF.Copy),
                         reads=[Bb["psBx%d" % pb_]], writes=[Bb["XT%d" % x_]])

        def stageC(e_):
            s_ = e_ % 2
            x_ = e_ % 2
            XTe, B_XT = XT[x_], Bb["XT%d" % x_]
            for j in range(8):
                for ch in range(2):
                    u_ = ch
                    cs_ = slice(ch * NC2, (ch + 1) * NC2)
                    for (ps_, fc, Bp) in ((psG[u_], j, Bb["psG%d" % u_]), (psU[u_], 8 + j, Bb["psU%d" % u_])):
                        for kc in range(8):
                            S.op("pe", lambda e, ps_=ps_, fc=fc, kc=kc, cs_=cs_: e.matmul(ps_[:, 0:NC2], lhsT=Wgu[s_][:, kc, fc * P:(fc + 1) * P], rhs=XTe[:, kc, cs_],
                                                                                      start=(kc == 0), stop=(kc == 7)),
                                 reads=[B_Wgu[s_], B_XT], writes=[Bp])
                    gs_, us_, es_, ts_ = gsb[u_], usb[u_], esb[u_], tsb[u_]
                    Bg, Bu, Be, Bt = Bb["gsb%d" % u_], Bb["usb%d" % u_], Bb["esb%d" % u_], Bb["tsb%d" % u_]
                    S.op("dve", lambda v, j=j, u_=u_, gs_=gs_: v.tensor_scalar(out=gs_[:], in0=psG[u_][:, 0:NC2], scalar1=bguT[:, j, e_:e_ + 1], scalar2=SW_LIMIT,
                                                                             op0=ALU.add, op1=ALU.min), reads=[Bb["psG%d" % u_], Bb["bguT"]], writes=[Bg])
                    S.op("act", lambda a, j=j, u_=u_, us_=us_: a.activation(out=us_[:], in_=psU[u_][:, 0:NC2], func=AF.Identity, bias=bguT[:, 8 + j, e_:e_ + 1]),
                         reads=[Bb["psU%d" % u_], Bb["bguT"]], writes=[Bu])
                    S.op("act", lambda a, gs_=gs_, es_=es_: a.activation(out=es_[:], in_=gs_[:], func=AF.Exp, scale=-SW_ALPHA), reads=[Bg], writes=[Be])
                    S.op("pool", lambda g, us_=us_: g.tensor_scalar(out=us_[:], in0=us_[:], scalar1=SW_LIMIT, scalar2=-SW_LIMIT, op0=ALU.min, op1=ALU.max),
                         reads=[Bu], writes=[Bu])
                    recip1p(S, "act", es_[:], es_[:], ts_[:], [Be], [Be], Bt)
                    S.op("pool", lambda g, gs_=gs_, es_=es_: g.tensor_tensor(out=gs_[:], in0=gs_[:], in1=es_[:], op=ALU.mult), reads=[Bg, Be], writes=[Bg])
                    S.op("dve", lambda v, j=j, gs_=gs_, us_=us_, cs_=cs_: v.scalar_tensor_tensor(out=actT[:, j, cs_], in0=us_[:], scalar=1.0, in1=gs_[:],
                                                                                                op0=ALU.add, op1=ALU.mult), reads=[Bg, Bu], writes=[Bb["actT"]])
            for rt in range(RT):
                ys = rt % 2
                for half in range(2):
                    for fc in range(8):
                        S.op("pe", lambda e, rt=rt, half=half, fc=fc: e.matmul(psD[half][:], lhsT=actT[:, fc, rt * P:(rt + 1) * P], rhs=Wd[s_][:, fc, half * 512:(half + 1) * 512],
                                                                               start=(fc == 0), stop=(fc == 7)),
                             reads=[Bb["actT"], B_Wd[s_]], writes=[Bb["psD%d" % half]])
                    if half:
                        S.op("act", lambda a, half=half, ys=ys: a.activation(out=yrow[ys][:, half * 512:(half + 1) * 512], in_=psD[half][:], func=AF.Copy),
                             reads=[Bb["psD%d" % half]], writes=[Bb["yrow%d" % ys]])
                    else:
                        S.op("dve", lambda v, half=half, ys=ys: v.tensor_copy(out=yrow[ys][:, half * 512:(half + 1) * 512], in_=psD[half][:]),
                             reads=[Bb["psD%d" % half]], writes=[Bb["yrow%d" % ys]])
                S.dma("sp", lambda e, rt=rt, ys=ys: e.dma_start(out=yg[e_ * CAP + rt * P:e_ * CAP + (rt + 1) * P, :], in_=yrow[ys][:]),
                      reads=[Bb["yrow%d" % ys]], writes=[B_yg])

        stageX(0)
        for e_ in range(n_exp):
            if e_ + 1 < n_exp:
                load_w(e_ + 1)
            LX = None
            if e_ + 1 < n_exp:
                S.begin_record()
                stageX(e_ + 1)
                LX = S.end_record()
            S.begin_record()
            stageC(e_)
            LC = S.end_record()
            S.play(LX, LC)
        S.barrier()
        C.release(mB)

        bd_pad = C.sb("bd_pad", [P, D], F32)
        G = [[C.sb("G%d_%d" % (q, k), [P, D], F32) for k in range(4)] for q in range(2)]
        acc = [C.sb("acc%d" % q, [P, D], F32) for q in range(2)]
        r2c = [C.sb("r2c%d" % q, [P, D], F32) for q in range(2)]
        outt = [C.sb("outt%d" % q, [P, D], F32) for q in range(2)]
        st3 = [C.sb("st3_%d" % q, [P, 12], F32) for q in range(2)]
        mv3 = [C.sb("mv3_%d" % q, [P, 4], F32) for q in range(2)]
        sc3 = [C.sb("sc3_%d" % q, [P, 2], F32) for q in range(2)]
        psC = [C.ps("psC%d" % i, [P, 512], F32) for i in range(2)]
        Bc = {n: Buf(n) for n in ["bd", "psC0", "psC1", "out"] + ["%s%d" % (n, q) for q in range(2) for n in ("G0_", "G1_", "G2_", "G3_", "acc", "r2c", "outt", "st3")]}
        S.op("pool", lambda g: g.memset(bd_pad[:], 0.0), writes=[Bc["bd"]])
        S.dma("sp", lambda e: e.dma_start(out=bd_pad[0:NEXP, :], in_=bdn), reads=[Bc["bd"]], writes=[Bc["bd"]])

        def comb_load(i):
            q = i % 2
            for k in range(4):
                S.dma("pool", lambda e, k=k: e.indirect_dma_start(out=G[q][k][:, :], out_offset=None, in_=yg[:, :],
                                                                  in_offset=bass.IndirectOffsetOnAxis(ap=slots_all[:, i, k:k + 1], axis=0),
                                                                  bounds_check=REG_SLOT, oob_is_err=False), reads=[B_yg, B_slots], writes=[Bc["G%d_%d" % (k, q)]])
            S.dma("sp", lambda e: e.dma_start(out=r2c[q][:], in_=rbuf[i * P:(i + 1) * P, :]), reads=[B_rbuf], writes=[Bc["r2c%d" % q]])

        def comb(i):
            q = i % 2
            for half in range(2):
                S.op("pe", lambda e, half=half: e.matmul(psC[half][:], lhsT=WT_all[:, i, :], rhs=bd_pad[:, half * 512:(half + 1) * 512], start=True, stop=True),
                     reads=[B_WT, Bc["bd"]], writes=[Bc["psC%d" % half]])
                S.op("dve", lambda v, half=half: v.tensor_tensor(out=acc[q][:, half * 512:(half + 1) * 512], in0=psC[half][:], in1=r2c[q][:, half * 512:(half + 1) * 512], op=ALU.add),
                     reads=[Bc["psC%d" % half], Bc["r2c%d" % q]], writes=[Bc["acc%d" % q]])
            for k in range(4):
                S.op("dve", lambda v, k=k: v.scalar_tensor_tensor(out=acc[q][:], in0=G[q][k][:], scalar=wts_all[:, i, k:k + 1], in1=acc[q][:], op0=ALU.mult, op1=ALU.add),
                     reads=[Bc["G%d_%d" % (k, q)], B_wts, Bc["acc%d" % q]], writes=[Bc["acc%d" % q]])
            layernorm_stats(S, acc[q][:], st3[q], mv3[q], sc3[q], Bc["acc%d" % q], Bc["st3%d" % q], LN_EPS)
            S.op("act", lambda a: a.activation(out=outt[q][:], in_=acc[q][:], func=AF.Identity, scale=sc3[q][:, 0:1], bias=sc3[q][:, 1:2]),
                 reads=[Bc["acc%d" % q], Bc["st3%d" % q]], writes=[Bc["outt%d" % q]])
            S.op("pool", lambda g: g.tensor_tensor(out=outt[q][:], in0=outt[q][:], in1=gbc2[:], op=ALU.mult), reads=[Bc["outt%d" % q], B_const], writes=[Bc["outt%d" % q]])
            S.op("pool", lambda g: g.tensor_tensor(out=outt[q][:], in0=outt[q][:], in1=bbc2[:], op=ALU.add), reads=[Bc["outt%d" % q], B_const], writes=[Bc["outt%d" % q]])
            S.dma("sp", lambda e: e.dma_start(out=out[i * P:(i + 1) * P, :], in_=outt[q][:]), reads=[Bc["outt%d" % q]], writes=[Bc["out"]])

        comb_load(0)
        for i in range(NT2):
            if i + 1 < NT2:
                comb_load(i + 1)
            comb(i)
        S.barrier()

    S.barrier()
    print("nops", S.nops, "ninst", S.ninst, {k: v for k, v in S.cnt.items() if k.startswith("E_")})
    C.release(0)
    S.close()
    return nc


def make_in_maps(inp):
    f32 = np.float32
    x = np.asarray(inp["x"], f32)
    p = np.asarray(inp["p"], f32)[0]
    w_in = np.asarray(inp["w_in"], f32)[0]
    hl = np.asarray(inp["hgrn_lb"], f32)
    maps = []
    for c in range(NCORES):
        b, hg = c // 4, c % 4
        a0, a1, gb = 2 * hg, 2 * hg + 1, hg
        A = lambda base, h: list(range(base + h * 128, base + (h + 1) * 128))
        Bv = lambda base, h: list(range(base + h * 256, base + (h + 1) * 256))
        cols = (A(1024, a0) + A(1024, a1) + A(4608, gb) +
                A(2048, a0) + A(2048, a1) + Bv(5120, gb) +
                A(3072, a0) + A(3072, a1) + Bv(6144, gb) +
                A(0, a0) + A(0, a1) + A(4096, gb) + list(range(7168, 7184)))
        w1 = np.ascontiguousarray(w_in[:, cols])
        vec = np.zeros((16, D), f32)
        vec[0] = inp["emb_ln_g"]; vec[1] = inp["emb_ln_b"]
        vec[2] = inp["ln_mix_g"][0]; vec[3] = inp["ln_mix_b"][0]
        vec[4] = inp["ln_moe_g"][0]; vec[5] = inp["ln_moe_b"][0]
        vec[6, 0:256] = hl[0, hg * 256:(hg + 1) * 256]
        vec[7, 0:256] = hl[1, hg * 256:(hg + 1) * 256]
        vec[8, 0:128] = inp["norm_a_g"][0]; vec[8, 128:256] = inp["norm_a_g"][0]; vec[8, 256:512] = inp["norm_b_g"][0]
        vec[9, 0:128] = inp["b_gla_up"][0][gb * 128:(gb + 1) * 128]
        vec[10, 0:NEXP] = inp["b_router"][0]
        seg = c % 4
        m = {
            "xb": x[b],
            "xseg": np.ascontiguousarray(x[b, seg * TSEG:(seg + 1) * TSEG]),
            "pseg": np.ascontiguousarray(p[b, seg * TSEG:(seg + 1) * TSEG]),
            "w1": w1,
            "oidx": np.ascontiguousarray(((b * 4 + np.arange(4))[None, None, :] * SEQ + seg * TSEG + (np.arange(TSEG // P) * P)[None, :, None]
                                          + np.arange(P)[:, None, None]).reshape(P, -1).astype(np.int32)),
            "wgate": np.ascontiguousarray(w_in[:, 7184:9232]),
            "vec": vec,
            "wgu": np.ascontiguousarray(np.asarray(inp["w_gla_up"], f32)[0][:, gb * 128:(gb + 1) * 128]),
            "wpa": np.asarray(inp["w_proj_a"], f32)[0],
            "wpb": np.asarray(inp["w_proj_b"], f32)[0],
            "wout": np.asarray(inp["w_out"], f32)[0],
            "wr": np.asarray(inp["w_router"], f32)[0],
            "wplg": np.asarray(inp["w_ple_gate"], f32)[0],
            "wplp": np.asarray(inp["w_ple_proj"], f32)[0],
            "wgup": np.asarray(inp["w_gate_up"], f32)[0],
            "bgup": np.asarray(inp["b_gate_up"], f32)[0],
            "wdn": np.asarray(inp["w_down"], f32)[0],
            "bdn": np.asarray(inp["b_down"], f32)[0],
        }
        maps.append(m)
    return maps


_CACHE = {}


def kernel(**inputs):
    if "nc" not in _CACHE:
        _CACHE["nc"] = build_program()
    nc = _CACHE["nc"]
    in_maps = make_in_maps(inputs)
    res = run_bass_kernel_spmd(nc, in_maps, core_ids=list(range(NCORES)))
    outp = np.empty((2, SEQ, D), np.float32)
    for c in range(NCORES):
        b, seg = c // 4, c % 4
        outp[b, seg * TSEG:(seg + 1) * TSEG] = res.results[c]["out"]
    return outp
```
